# Optimizing a Trainium2 kernel written in Bass

```python
import jax
import jax.numpy as jnp
from jax import lax
import numpy as np


D_MODEL = 2048
BATCH = 2
SEQ = 16384
DEPTH = 2

BLOCK = 128
ROPE_THETA = 10000.0
LN_EPS = 1e-5
RMS_EPS = 1e-6
PLE_DIM = 256
SWA_HEADS = 32
SWA_KV_HEADS = 4
SWA_GROUP = SWA_HEADS // SWA_KV_HEADS
SWA_HEAD_DIM = 64
SWA_WINDOW = 128
MLA_HEADS = 16
MLA_NOPE = 128
MLA_ROPE = 64
MLA_V = 128
MLA_LATENT = 512
MLA_SCALE = (MLA_NOPE + MLA_ROPE) ** -0.5
IDX_HEADS = 16
IDX_DIM = 64
IDX_ROPE = 32
DSA_TOPK = 256
DSA_PARTS = (MLA_HEADS * (MLA_NOPE + MLA_ROPE), MLA_LATENT, MLA_ROPE, IDX_HEADS * IDX_DIM, IDX_DIM, IDX_HEADS)
DSA_IN = sum(DSA_PARTS)
DSA_SPLITS = tuple(int(v) for v in np.cumsum(DSA_PARTS)[:-1])
FFN_DIM = 5632
MOE_EXPERTS = 8
MOE_TOP_K = 2
MOE_FFN_DIM = 7168
MOE_BLOCK = 512

kernel_name = 'hybrid_swa_dsa_deepnorm_moe'


def layer_norm(x, g, b):
    xf = x.astype(jnp.float32)
    mu = jnp.mean(xf, axis=-1, keepdims=True)
    var = jnp.mean(jnp.square(xf - mu), axis=-1, keepdims=True)
    return ((xf - mu) * lax.rsqrt(var + LN_EPS) * g + b).astype(x.dtype)


def rms_norm(x, g):
    xf = x.astype(jnp.float32)
    return (xf * lax.rsqrt(jnp.mean(jnp.square(xf), axis=-1, keepdims=True) + RMS_EPS) * g).astype(x.dtype)


def rope(x, pos):
    d = x.shape[-1]
    inv_freq = ROPE_THETA ** (-jnp.arange(0, d, 2, dtype=jnp.float32) / d)
    ang = pos.astype(jnp.float32)[..., None] * inv_freq
    cos = jnp.cos(ang)[:, :, None, :]
    sin = jnp.sin(ang)[:, :, None, :]
    xf = x.astype(jnp.float32)
    x1, x2 = xf[..., : d // 2], xf[..., d // 2:]
    return jnp.concatenate([x1 * cos - x2 * sin, x2 * cos + x1 * sin], axis=-1).astype(x.dtype)


def _to_blocks(a):
    b, s = a.shape[:2]
    return jnp.moveaxis(a.reshape((b, s // BLOCK, BLOCK) + a.shape[2:]), 1, 0)


def _from_blocks(a):
    nb, b, bq = a.shape[:3]
    return jnp.moveaxis(a, 0, 1).reshape((b, nb * bq) + a.shape[3:])


def swa_mixer(x, pos, w_qkv, sinks, w_o):
    b, s, _ = x.shape
    nb = s // BLOCK
    qkv = x @ w_qkv
    q, k, v = jnp.split(qkv, [SWA_HEADS * SWA_HEAD_DIM, (SWA_HEADS + SWA_KV_HEADS) * SWA_HEAD_DIM], axis=-1)
    q = rope(q.reshape(b, s, SWA_HEADS, SWA_HEAD_DIM), pos).reshape(b, s, SWA_KV_HEADS, SWA_GROUP, SWA_HEAD_DIM)
    k = rope(k.reshape(b, s, SWA_KV_HEADS, SWA_HEAD_DIM), pos)
    v = v.reshape(b, s, SWA_KV_HEADS, SWA_HEAD_DIM)

    def band(a):
        ab = a.reshape(b, nb, BLOCK, SWA_KV_HEADS, SWA_HEAD_DIM)
        prev = jnp.concatenate([jnp.zeros_like(ab[:, :1]), ab[:, :-1]], axis=1)
        return jnp.moveaxis(jnp.concatenate([prev, ab], axis=2), 1, 0)

    q_pos = jnp.arange(s).reshape(nb, BLOCK)
    k_pos = q_pos[:, :1] - BLOCK + jnp.arange(2 * BLOCK)[None, :]
    sink = sinks.astype(jnp.float32).reshape(SWA_KV_HEADS, SWA_GROUP)
    scale = SWA_HEAD_DIM ** -0.5

    def attend(args):
        qb, kb, vb, qp, kp = args
        logits = jnp.einsum('bqkgd,bskd->bkgqs', qb, kb).astype(jnp.float32) * scale
        rel = qp[:, None] - kp[None, :]
        allowed = (rel >= 0) & (rel < SWA_WINDOW) & (kp[None, :] >= 0)
        logits = jnp.where(allowed, logits, -jnp.inf)
        sink_col = jnp.broadcast_to(sink[None, :, :, None, None], logits.shape[:-1] + (1,))
        probs = jax.nn.softmax(jnp.concatenate([logits, sink_col], axis=-1), axis=-1)[..., :-1]
        return jnp.einsum('bkgqs,bskd->bqkgd', probs.astype(vb.dtype), vb)

    o = lax.map(attend, (_to_blocks(q), band(k), band(v), q_pos, k_pos))
    o = _from_blocks(o).reshape(b, s, SWA_HEADS * SWA_HEAD_DIM)
    return o @ w_o


def dsa_mixer(x, pos, w_in, kv_norm, w_uk, w_uv, w_o):
    b, s, _ = x.shape
    nb = s // BLOCK
    n_sel = min(DSA_TOPK, s // 4)
    h = x @ w_in
    q, c_kv, k_r, q_i, k_i, w_i = jnp.split(h, DSA_SPLITS, axis=-1)
    q = q.reshape(b, s, MLA_HEADS, MLA_NOPE + MLA_ROPE)
    q_nope = q[..., :MLA_NOPE]
    q_rope = rope(q[..., MLA_NOPE:], pos)
    c_kv = rms_norm(c_kv, kv_norm)
    k_r = rope(k_r[:, :, None, :], pos)[:, :, 0]
    q_i = q_i.reshape(b, s, IDX_HEADS, IDX_DIM)
    q_i = jnp.concatenate([rope(q_i[..., :IDX_ROPE], pos), q_i[..., IDX_ROPE:]], axis=-1)
    k_i = jnp.concatenate([rope(k_i[:, :, None, :IDX_ROPE], pos)[:, :, 0], k_i[..., IDX_ROPE:]], axis=-1)
    w_i = w_i * IDX_HEADS ** -0.5
    key_pos = jnp.arange(s)
    b_ix = jnp.arange(b)[:, None, None]

    def attend(args):
        qn, qr, qi, wi, qp = args
        idx_logits = jnp.einsum('bqhd,bsd->bqhs', qi, k_i).astype(jnp.float32) * IDX_DIM ** -0.5
        score = jnp.einsum('bqh,bqhs->bqs', wi.astype(jnp.float32), jax.nn.relu(idx_logits))
        score = jnp.where(key_pos[None, None, :] <= qp[None, :, None], score, -jnp.inf)
        _, sel = lax.top_k(score, n_sel)
        valid = sel <= qp[None, :, None]
        c_sel = c_kv[b_ix, sel]
        kr_sel = k_r[b_ix, sel]
        q_lat = jnp.einsum('bqhn,hcn->bqhc', qn, w_uk)
        logits = (jnp.einsum('bqhc,bqkc->bqhk', q_lat, c_sel)
                  + jnp.einsum('bqhr,bqkr->bqhk', qr, kr_sel)).astype(jnp.float32) * MLA_SCALE
        logits = jnp.where(valid[:, :, None, :], logits, -jnp.inf)
        probs = jax.nn.softmax(logits, axis=-1).astype(c_sel.dtype)
        o_lat = jnp.einsum('bqhk,bqkc->bqhc', probs, c_sel)
        return jnp.einsum('bqhc,hcv->bqhv', o_lat, w_uv)

    o = lax.map(attend, (_to_blocks(q_nope), _to_blocks(q_rope), _to_blocks(q_i), _to_blocks(w_i),
                         key_pos.reshape(nb, BLOCK)))
    return _from_blocks(o).reshape(b, s, MLA_HEADS * MLA_V) @ w_o


def swiglu(x, w1, w3, w2):
    return (jax.nn.silu(x @ w1) * (x @ w3)) @ w2


def moe_swiglu(h, w_router, w1, w3, w2):
    t, d = h.shape
    n = t * MOE_TOP_K
    logits = (h @ w_router).astype(jnp.float32)
    top_val, top_idx = lax.top_k(logits, MOE_TOP_K)
    gates = jax.nn.softmax(top_val, axis=-1).astype(h.dtype)
    e_flat = top_idx.reshape(n)
    tok_flat = jnp.arange(n, dtype=jnp.int32) // MOE_TOP_K
    g_flat = gates.reshape(n)
    order = jnp.argsort(e_flat)
    e_sorted = e_flat[order]
    counts = jnp.bincount(e_flat, length=MOE_EXPERTS)
    starts = jnp.cumsum(counts) - counts
    padded = (counts + MOE_BLOCK - 1) // MOE_BLOCK * MOE_BLOCK
    pends = jnp.cumsum(padded)
    pstarts = pends - padded
    dest = pstarts[e_sorted] + (jnp.arange(n) - starts[e_sorted])
    n_blocks = (n + MOE_EXPERTS * (MOE_BLOCK - 1) + MOE_BLOCK - 1) // MOE_BLOCK
    n_slots = n_blocks * MOE_BLOCK
    slot_tok = jnp.zeros((n_slots,), jnp.int32).at[dest].set(tok_flat[order])
    slot_gate = jnp.zeros((n_slots,), h.dtype).at[dest].set(g_flat[order])
    blk_start = jnp.arange(n_blocks, dtype=pends.dtype) * MOE_BLOCK
    blk_expert = jnp.minimum(jnp.searchsorted(pends, blk_start, side='right'), MOE_EXPERTS - 1)

    def run_block(args):
        tok, e = args
        xb = h[tok]
        return (jax.nn.silu(xb @ w1[e]) * (xb @ w3[e])) @ w2[e]

    y = lax.map(run_block, (slot_tok.reshape(n_blocks, MOE_BLOCK), blk_expert)).reshape(n_slots, d)
    return jax.ops.segment_sum(y * slot_gate[:, None], slot_tok, num_segments=t)


def setup_inputs(seed: int = 0) -> dict:
    key = jax.random.key(seed)
    ks = jax.random.split(key, 24)
    n_a = (DEPTH + 1) // 2
    n_b = DEPTH // 2
    beta = (8.0 * DEPTH) ** -0.25

    def nrm(k, shape, scale):
        return jax.random.normal(k, shape, jnp.float32) * scale

    qkv_out = (SWA_HEADS + 2 * SWA_KV_HEADS) * SWA_HEAD_DIM
    offset = jax.random.randint(ks[2], (BATCH, 1), 0, 1024, dtype=jnp.int32)
    return {
        'x': nrm(ks[0], (BATCH, SEQ, D_MODEL), 1.0),
        'p': nrm(ks[1], (DEPTH, BATCH, SEQ, PLE_DIM), 1.0),
        'positions': offset + jnp.arange(SEQ, dtype=jnp.int32)[None, :],
        'swa_w_qkv': nrm(ks[3], (n_a, D_MODEL, qkv_out), D_MODEL ** -0.5),
        'swa_sinks': nrm(ks[4], (n_a, SWA_HEADS), 0.5),
        'swa_w_o': nrm(ks[5], (n_a, SWA_HEADS * SWA_HEAD_DIM, D_MODEL), beta * (SWA_HEADS * SWA_HEAD_DIM) ** -0.5),
        'dense_w1': nrm(ks[6], (n_a, D_MODEL, FFN_DIM), D_MODEL ** -0.5),
        'dense_w3': nrm(ks[7], (n_a, D_MODEL, FFN_DIM), D_MODEL ** -0.5),
        'dense_w2': nrm(ks[8], (n_a, FFN_DIM, D_MODEL), beta * FFN_DIM ** -0.5),
        'dsa_w_in': nrm(ks[9], (n_b, D_MODEL, DSA_IN), D_MODEL ** -0.5),
        'dsa_kv_norm': 1.0 + nrm(ks[10], (n_b, MLA_LATENT), 0.02),
        'dsa_w_uk': nrm(ks[11], (n_b, MLA_HEADS, MLA_LATENT, MLA_NOPE), MLA_LATENT ** -0.5),
        'dsa_w_uv': nrm(ks[12], (n_b, MLA_HEADS, MLA_LATENT, MLA_V), MLA_LATENT ** -0.5),
        'dsa_w_o': nrm(ks[13], (n_b, MLA_HEADS * MLA_V, D_MODEL), beta * (MLA_HEADS * MLA_V) ** -0.5),
        'moe_router': nrm(ks[14], (n_b, D_MODEL, MOE_EXPERTS), D_MODEL ** -0.5),
        'moe_w1': nrm(ks[15], (n_b, MOE_EXPERTS, D_MODEL, MOE_FFN_DIM), D_MODEL ** -0.5),
        'moe_w3': nrm(ks[16], (n_b, MOE_EXPERTS, D_MODEL, MOE_FFN_DIM), D_MODEL ** -0.5),
        'moe_w2': nrm(ks[17], (n_b, MOE_EXPERTS, MOE_FFN_DIM, D_MODEL), beta * MOE_FFN_DIM ** -0.5),
        'ln_g': 1.0 + nrm(ks[18], (DEPTH, 2, D_MODEL), 0.02),
        'ln_b': nrm(ks[19], (DEPTH, 2, D_MODEL), 0.02),
        'ple_w_p': nrm(ks[20], (DEPTH, PLE_DIM, D_MODEL), PLE_DIM ** -0.5),
        'ple_w_g': nrm(ks[21], (DEPTH, D_MODEL, D_MODEL), D_MODEL ** -0.5),
    }


def reference(x, p, positions, swa_w_qkv, swa_sinks, swa_w_o, dense_w1, dense_w3, dense_w2,
              dsa_w_in, dsa_kv_norm, dsa_w_uk, dsa_w_uv, dsa_w_o, moe_router, moe_w1, moe_w3, moe_w2,
              ln_g, ln_b, ple_w_p, ple_w_g):
    alpha = (2.0 * DEPTH) ** 0.25
    for i in range(DEPTH):
        j = i // 2
        if i % 2 == 0:
            mix = swa_mixer(x, positions, swa_w_qkv[j], swa_sinks[j], swa_w_o[j])
        else:
            mix = dsa_mixer(x, positions, dsa_w_in[j], dsa_kv_norm[j], dsa_w_uk[j], dsa_w_uv[j], dsa_w_o[j])
        x = layer_norm(alpha * x + mix, ln_g[i, 0], ln_b[i, 0])
        if i % 2 == 0:
            ff = swiglu(x, dense_w1[j], dense_w3[j], dense_w2[j])
        else:
            ff = moe_swiglu(x.reshape(-1, x.shape[-1]), moe_router[j], moe_w1[j], moe_w3[j], moe_w2[j]).reshape(x.shape)
        x = layer_norm(alpha * x + ff, ln_g[i, 1], ln_b[i, 1])
        x = x + jax.nn.sigmoid(x @ ple_w_g[i]) * (p[i] @ ple_w_p[i])
    return x
```

```python
import math
from contextlib import ExitStack
import numpy as np
import concourse.bass as bass
import concourse.mybir as mybir

F32 = mybir.dt.float32
BF16 = mybir.dt.bfloat16
I32 = mybir.dt.int32
AF = mybir.ActivationFunctionType
ALU = mybir.AluOpType
AX = mybir.AxisListType

D = 2048
ALPHA = (2.0 * 2) ** 0.25
LN_EPS = 1e-5
import os
SAME_ENGINE_SYNC = bool(int(os.environ.get('SES', '1')))


class T:
    def __init__(self, t, name=""):
        self.t = t
        self.name = name
        self.w = None
        self.r = {}

    def __getitem__(self, k):
        return self.t[k]


class Sy:
    EPOCH = 12000

    def __init__(self, nc, stack):
        self.nc = nc
        self.stack = stack
        self.q = {'pe': nc.tensor, 'act': nc.scalar, 'dve': nc.vector, 'pool': nc.gpsimd, 'sp': nc.sync}
        self.st = {}
        self.seen = {k: {} for k in self.q}
        self.nsem = 0
        self.ninst = 0

    def _stream(self, key, step):
        if key not in self.st:
            self.st[key] = [0, step, []]
        return self.st[key]

    def _sem(self, key, c):
        st = self.st[key]
        E = self.EPOCH // st[1]
        return self._sem2(key, c, E)

    def _sem2(self, key, c, E):
        st = self.st[key]
        ep = (c - 1) // E
        while len(st[2]) <= ep:
            st[2].append(self.stack.enter_context(self.nc.semaphore(f"s_{key}_{len(st[2])}")))
            self.nsem += 1
        return st[2][ep], ((c - 1) % E + 1) * st[1]

    def op(self, q, fn, reads=(), writes=(), stream=None, step=1, signal=True):
        key = stream if stream is not None else q
        st = self._stream(key, step)
        needs = {}
        for b in reads:
            if b.w is not None:
                k, c = b.w
                if needs.get(k, 0) < c:
                    needs[k] = c
        for b in writes:
            if b.w is not None:
                k, c = b.w
                if needs.get(k, 0) < c:
                    needs[k] = c
            for k, c in b.r.items():
                if needs.get(k, 0) < c:
                    needs[k] = c
        eng = self.q[q]
        for k, c in needs.items():
            if k == key and stream is None and (not SAME_ENGINE_SYNC or q == 'pe'):
                continue
            if self.seen[q].get(k, 0) >= c:
                continue
            sem, val = self._sem(k, c)
            eng.wait_ge(sem, val)
            self.seen[q][k] = c
            self.ninst += 1
        ins = fn()
        self.ninst += 1
        c = st[0] + 1
        if signal:
            st[0] = c
            sem, _ = self._sem(key, c)
            ins.then_inc(sem, st[1])
        for b in reads:
            if b.r.get(key, 0) < c:
                b.r[key] = c
        for b in writes:
            b.w = (key, c)
            b.r = {}
        return ins

    def final_wait(self, q, bufs):
        eng = self.q[q]
        for b in bufs:
            items = dict(b.r)
            if b.w is not None:
                items[b.w[0]] = max(items.get(b.w[0], 0), b.w[1])
            for k, c in items.items():
                sem, val = self._sem(k, c)
                eng.wait_ge(sem, val)


class Ctx:
    def __init__(self):
        self.nc = bass.Bass("TRN2", target_bir_lowering=False)
        self.stack = ExitStack()
        self.sy = Sy(self.nc, self.stack)
        self.banks = []
        self.bank_i = 0
        self.dq_i = 0

    def sb(self, name, shape, dt=F32):
        return self.stack.enter_context(self.nc.sbuf_tensor(name, list(shape), dt))

    def tsb(self, name, shape, dt=F32):
        return T(self.sb(name, shape, dt), name)

    def init_psum(self):
        for i in range(8):
            t = self.stack.enter_context(self.nc.psum_tensor(f"ps{i}", [128, 512], F32))
            self.banks.append(T(t, f"ps{i}"))

    def bank(self):
        b = self.banks[self.bank_i % 8]
        self.bank_i += 1
        return b

    def din(self, name, shape, dt=F32):
        return self.nc.dram_tensor(name, list(shape), dt, kind="ExternalInput").ap()

    def dout(self, name, shape, dt=F32):
        return self.nc.dram_tensor(name, list(shape), dt, kind="ExternalOutput").ap()

    def dma(self, out_ap, in_ap, wr=(), rd=(), q='sp', stream=None):
        nc = self.nc
        key = stream if stream is not None else ("d_" + (wr[0].name if wr else rd[0].name))
        eng = self.sy.q[q]
        return self.sy.op(q, lambda: eng.dma_start(out=out_ap, in_=in_ap), reads=rd, writes=wr, stream=key, step=16)

    def mm(self, out_ap, lhsT, rhs, start, stop, rd=(), wr=()):
        nc = self.nc
        return self.sy.op('pe', lambda: nc.tensor.matmul(out_ap, lhsT, rhs, start=start, stop=stop),
                          reads=rd, writes=wr, signal=stop)

    def tr(self, out_ap, in_ap, ident_ap, rd=(), wr=(), signal=True):
        nc = self.nc
        return self.sy.op('pe', lambda: nc.tensor.transpose(out_ap, in_ap, ident_ap), reads=rd, writes=wr, signal=signal)

    def act(self, out_ap, in_ap, func, rd=(), wr=(), **kw):
        nc = self.nc
        return self.sy.op('act', lambda: nc.scalar.activation(out=out_ap, in_=in_ap, func=func, **kw), reads=rd, writes=wr)

    def v(self, q, name, *args, rd=(), wr=(), **kw):
        eng = self.sy.q[q]
        return self.sy.op(q, lambda: getattr(eng, name)(*args, **kw), reads=rd, writes=wr)


def layer_norm_tile(cx, xt, xap, gB, bB, scr):
    stats, mv, rstd = scr['stats'], scr['mv'], scr['rstd']
    for c in range(4):
        cx.v('dve', 'bn_stats', stats.t[:, c, :], xap[:, c * 512:(c + 1) * 512], rd=[xt], wr=[stats])
    cx.v('dve', 'bn_aggr', mv.t[:, :], stats.t[:, :, :], rd=[stats], wr=[mv])
    cx.v('dve', 'tensor_scalar', rstd.t[:, :], mv.t[:, 1:2], LN_EPS, None, ALU.add, rd=[mv], wr=[rstd])
    cx.act(rstd.t[:, :], rstd.t[:, :], AF.Sqrt, rd=[rstd], wr=[rstd])
    cx.v('dve', 'reciprocal', rstd.t[:, :], rstd.t[:, :], rd=[rstd], wr=[rstd])
    cx.v('dve', 'tensor_scalar', xap, xap, mv.t[:, 0:1], rstd.t[:, 0:1], ALU.subtract, ALU.mult, rd=[xt, mv, rstd], wr=[xt])
    cx.v('dve', 'tensor_tensor', xap, xap, gB.t[:, :], ALU.mult, rd=[xt, gB], wr=[xt])
    cx.v('pool', 'tensor_tensor', xap, xap, bB.t[:, :], ALU.add, rd=[xt, bB], wr=[xt])


def transpose_group(cx, src_t, src, dstT_t, dstT, ident, ntile=4, nk=16):
    for k in range(nk):
        ps = cx.bank()
        for n in range(ntile):
            cx.tr(ps.t[:, n * 128:(n + 1) * 128], src[:, n, k * 128:(k + 1) * 128], ident.t[:, :],
                  rd=[src_t, ident], wr=[ps], signal=(n == ntile - 1))
        if k % 2 == 0:
            cx.v('dve', 'tensor_copy', dstT[:, k, 0:ntile * 128], ps.t[:, 0:ntile * 128], rd=[ps], wr=[dstT_t])
        else:
            cx.act(dstT[:, k, 0:ntile * 128], ps.t[:, 0:ntile * 128], AF.Copy, rd=[ps], wr=[dstT_t])


def ffn_slabs(cx, xT_t, xT, acc_t, acc, w1, w3, w2, F, wbufs, gbuf, sil, gates=None, first_scale=None, ntile=4, gates_t=None):
    NTOK = ntile * 128
    SL = 256
    nsl = F // SL
    w1v = w1.rearrange("(k p) f -> p k f", p=128)
    w3v = w3.rearrange("(k p) f -> p k f", p=128)
    w2v = w2.rearrange("(c p) o -> p c o", p=128)
    first = first_scale is not None
    for s in range(nsl):
        wa, wb, wc = wbufs['a'][s % 2], wbufs['b'][s % 2], wbufs['c'][s % 2]
        cx.dma(wa.t[:, :, :], w1v[:, :, s * SL:(s + 1) * SL], wr=[wa], q='pool')
        cx.dma(wb.t[:, :, :], w3v[:, :, s * SL:(s + 1) * SL], wr=[wb], q='pool')
        cx.dma(wc.t[:, :, :], w2v[:, s * 2:(s + 1) * 2, :], wr=[wc], q='pool')
        g = gbuf[s % 2]
        for fc in range(2):
            pu = cx.bank()
            for k in range(16):
                cx.mm(pu.t[:, 0:NTOK], wa.t[:, k, fc * 128:(fc + 1) * 128], xT[:, k, 0:NTOK], k == 0, k == 15,
                      rd=[wa, xT_t], wr=[pu])
            pv = cx.bank()
            for k in range(16):
                cx.mm(pv.t[:, 0:NTOK], wb.t[:, k, fc * 128:(fc + 1) * 128], xT[:, k, 0:NTOK], k == 0, k == 15,
                      rd=[wb, xT_t], wr=[pv])
            sl = sil[fc]
            cx.act(sl.t[:, 0:NTOK], pu.t[:, 0:NTOK], AF.Silu, rd=[pu], wr=[sl])
            cx.v('dve', 'tensor_tensor', g.t[:, fc, 0:NTOK], sl.t[:, 0:NTOK], pv.t[:, 0:NTOK], ALU.mult, rd=[sl, pv], wr=[g])
        for n in range(ntile):
            for o in range(4):
                py = cx.bank()
                for fc in range(2):
                    cx.mm(py.t[:, :], g.t[:, fc, n * 128:(n + 1) * 128], wc.t[:, fc, o * 512:(o + 1) * 512],
                          fc == 0, fc == 1, rd=[g, wc], wr=[py])
                dst = acc[:, n, o * 512:(o + 1) * 512]
                if gates is not None:
                    if first:
                        cx.v('dve', 'tensor_scalar', dst, dst, first_scale, None, ALU.mult, rd=[acc_t], wr=[acc_t])
                    cx.v('dve', 'scalar_tensor_tensor', dst, py.t[:, :], gates[:, n:n + 1], dst, ALU.mult, ALU.add,
                         rd=[py, acc_t] + ([gates_t] if gates_t is not None else []), wr=[acc_t])
                elif first:
                    cx.v('dve', 'scalar_tensor_tensor', dst, dst, first_scale, py.t[:, :], ALU.mult, ALU.add,
                         rd=[py, acc_t], wr=[acc_t])
                else:
                    cx.v('dve', 'tensor_tensor', dst, dst, py.t[:, :], ALU.add, rd=[py, acc_t], wr=[acc_t])
        first = False


def ple_block(cx, x2_t, x2, x2T_t, x2T, p_dram_tiles, wg, wp, ident, bufs, ntile=4):
    pin, pT, sg, tmp = bufs['pin'], bufs['pT'], bufs['sg'], bufs['tmp']
    wgbufs = bufs['wa']
    cx.dma(pin.t[:, 0:ntile, :], p_dram_tiles, wr=[pin])
    for k in range(2):
        ps = cx.bank()
        for n in range(ntile):
            cx.tr(ps.t[:, n * 128:(n + 1) * 128], pin.t[:, n, k * 128:(k + 1) * 128], ident.t[:, :],
                  rd=[pin, ident], wr=[ps], signal=(n == ntile - 1))
        cx.v('dve', 'tensor_copy', pT.t[:, k, 0:ntile * 128], ps.t[:, 0:ntile * 128], rd=[ps], wr=[pT])
    wgv = wg.rearrange("(k p) o -> p k o", p=128)
    wpv = wp.rearrange("(k p) o -> p k o", p=128)
    for s in range(8):
        wa = wgbufs[s % 2]
        cx.dma(wa.t[:, :, :], wgv[:, :, s * 256:(s + 1) * 256], wr=[wa], q='pool')
        wpb = bufs['wp'][s % 2]
        cx.dma(wpb.t[:, :, :], wpv[:, :, s * 256:(s + 1) * 256], wr=[wpb], q='pool')
        for n in range(ntile):
            pg = cx.bank()
            for k in range(16):
                cx.mm(pg.t[:, 0:256], x2T[:, k, n * 128:(n + 1) * 128], wa.t[:, k, :], k == 0, k == 15,
                      rd=[x2T_t, wa], wr=[pg])
            for k in range(2):
                cx.mm(pg.t[:, 256:512], pT.t[:, k, n * 128:(n + 1) * 128], wpb.t[:, k, :], k == 0, k == 1,
                      rd=[pT, wpb], wr=[pg])
            cx.act(sg.t[:, :], pg.t[:, 0:256], AF.Sigmoid, rd=[pg], wr=[sg])
            cx.v('dve', 'tensor_tensor', tmp.t[:, :], sg.t[:, :], pg.t[:, 256:512], ALU.mult, rd=[sg, pg], wr=[tmp])
            dst = x2[:, n, s * 256:(s + 1) * 256]
            cx.v('pool', 'tensor_tensor', dst, dst, tmp.t[:, :], ALU.add, rd=[tmp, x2_t], wr=[x2_t])


def rope_tables(cx, pos_i, t0, posf, invf, cos2, sin2, scr_ang, scr_y, scr_i, scr_f, nf=32):
    TWO_PI = 2.0 * math.pi
    NT = 4
    cx.v('dve', 'tensor_copy', posf.t[:, :], pos_i.t[:, t0:t0 + 4], rd=[pos_i], wr=[posf])
    cx.v('dve', 'tensor_tensor', scr_ang.t[:, :, :], invf.t[:, :].unsqueeze(1).to_broadcast([128, NT, nf]),
         posf.t[:, :].unsqueeze(2).to_broadcast([128, NT, nf]), ALU.mult, rd=[invf, posf], wr=[scr_ang])
    for (dst, off) in ((sin2, 0.0), (cos2, 0.25)):
        cx.v('dve', 'tensor_scalar', scr_y.t[:, :, :], scr_ang.t[:, :, :], 1.0 / TWO_PI, off, ALU.mult, ALU.add,
             rd=[scr_ang], wr=[scr_y])
        cx.v('dve', 'tensor_copy', scr_i.t[:, :, :], scr_y.t[:, :, :], rd=[scr_y], wr=[scr_i])
        cx.v('dve', 'tensor_copy', scr_f.t[:, :, :], scr_i.t[:, :, :], rd=[scr_i], wr=[scr_f])
        cx.v('dve', 'tensor_tensor', scr_y.t[:, :, :], scr_y.t[:, :, :], scr_f.t[:, :, :], ALU.subtract, rd=[scr_y, scr_f], wr=[scr_y])
        cx.v('dve', 'tensor_scalar', scr_f.t[:, :, :], scr_y.t[:, :, :], 0.5, None, ALU.is_gt, rd=[scr_y], wr=[scr_f])
        cx.v('dve', 'tensor_tensor', scr_y.t[:, :, :], scr_y.t[:, :, :], scr_f.t[:, :, :], ALU.subtract, rd=[scr_y, scr_f], wr=[scr_y])
        cx.v('dve', 'tensor_scalar', scr_y.t[:, :, :], scr_y.t[:, :, :], TWO_PI, 3.14159, ALU.mult, ALU.min,
             rd=[scr_y], wr=[scr_y])
        cx.v('dve', 'tensor_scalar', scr_y.t[:, :, :], scr_y.t[:, :, :], -3.14159, None, ALU.max,
             rd=[scr_y], wr=[scr_y])
        cx.act(dst.t[:, :, :], scr_y.t[:, :, :], AF.Sin, rd=[scr_y], wr=[dst])


def rope_apply(cx, ps, nh, cos2, sin2, tile, dst_t, dst, tA, tB):
    X = ps.t[:, 0:nh * 64].rearrange("p (h two d) -> p h two d", two=2, d=32)
    cb = cos2.t[:, tile, :].unsqueeze(1).unsqueeze(1).to_broadcast([128, nh, 2, 32])
    sbb = sin2.t[:, tile, :].unsqueeze(1).unsqueeze(1).to_broadcast([128, nh, 2, 32])
    cx.v('dve', 'tensor_tensor', tA.t[:, 0:nh, :].rearrange("p h (two d) -> p h two d", two=2), X, cb, ALU.mult, rd=[ps, cos2], wr=[tA])
    cx.v('dve', 'tensor_tensor', tB.t[:, 0:nh, :].rearrange("p h (two d) -> p h two d", two=2), X, sbb, ALU.mult, rd=[ps, sin2], wr=[tB])
    cx.v('pool', 'tensor_tensor', dst[:, :, 0:32], tA.t[:, 0:nh, 0:32], tB.t[:, 0:nh, 32:64], ALU.subtract,
         rd=[tA, tB], wr=[dst_t])
    cx.v('pool', 'tensor_tensor', dst[:, :, 32:64], tA.t[:, 0:nh, 32:64], tB.t[:, 0:nh, 0:32], ALU.add,
         rd=[tA, tB], wr=[dst_t])


def build_l1(NT):
    cx = Ctx()
    nc = cx.nc
    NG = NT // 4
    TOK = NT * 128
    x_own = cx.din("x_own", [TOK, D]); x_prev = cx.din("x_prev", [TOK, D])
    pos_own = cx.din("pos_own", [128, NT], I32); pos_prev = cx.din("pos_prev", [128, NT], I32)
    pvalid = cx.din("pvalid", [128, NT])
    p0 = cx.din("p0", [TOK, 256])
    wqkv = cx.din("wqkv", [D, 2560]); sinks = cx.din("sinks", [1, 32]); wo = cx.din("wo", [D, D])
    w1 = cx.din("w1", [D, 5632]); w3 = cx.din("w3", [D, 5632]); w2 = cx.din("w2", [5632, D])
    lng = cx.din("lng", [2, D]); lnb = cx.din("lnb", [2, D])
    wp = cx.din("wp", [256, D]); wg = cx.din("wg", [D, D])
    c_ident = cx.din("c_ident", [128, 128]); c_invf = cx.din("c_invf", [128, 32])
    c_mprev = cx.din("c_mprev", [128, 128]); c_mown = cx.din("c_mown", [128, 128])
    x1 = cx.dout("x1", [TOK, D])
    cx.init_psum()
    xo = cx.tsb("xo", [128, 4, D]); xpb = [cx.tsb(f"xpb{i}", [128, D], BF16) for i in range(1)] * 2
    xTa = cx.tsb("xTa", [128, 16, 512], BF16); xTp = cx.tsb("xTp", [128, 16, 512], BF16)
    wa = [cx.tsb(f"wa{i}", [128, 16, 256], BF16) for i in range(2)]
    wb = [cx.tsb(f"wb{i}", [128, 16, 256], BF16) for i in range(2)]
    wc = [cx.tsb(f"wc{i}", [128, 2, D], BF16) for i in range(2)]
    qT = cx.tsb("qT", [128, 16, 512], BF16); attnT = xTp
    kTo = cx.tsb("kTo", [128, 2, 512], BF16); kTp = cx.tsb("kTp", [128, 2, 512], BF16)
    Vo = cx.tsb("Vo", [128, 4, 4, 128], BF16); Vp = cx.tsb("Vp", [128, 4, 4, 128], BF16)
    gB = cx.tsb("gB", [128, D]); bB = cx.tsb("bB", [128, D])
    ident = cx.tsb("ident", [128, 128]); identb = cx.tsb("identb", [128, 128], BF16)
    invf = cx.tsb("invf", [128, 32])
    mprev = cx.tsb("mprev", [128, 128], BF16); mown = cx.tsb("mown", [128, 128], BF16)
    mtmp = cx.tsb("mtmp", [128, 128])
    posi_o = cx.tsb("posi_o", [128, NT], I32); posi_p = cx.tsb("posi_p", [128, NT], I32)
    posf = cx.tsb("posf", [128, 4])
    cos_o = cx.tsb("cos_o", [128, 4, 32]); sin_o = cx.tsb("sin_o", [128, 4, 32])
    cos_p = cx.tsb("cos_p", [128, 4, 32]); sin_p = cx.tsb("sin_p", [128, 4, 32])
    sang = cx.tsb("sang", [128, 4, 32]); sy_ = cx.tsb("sy", [128, 4, 32]); si_ = cx.tsb("si", [128, 4, 32], I32); sf_ = cx.tsb("sf", [128, 4, 32])
    pv = cx.tsb("pv", [128, NT]); pvmat = cx.tsb("pvmat", [128, NT, 128], BF16); ones = cx.tsb("ones", [128, 128], BF16)
    sk = cx.tsb("sk", [128, 32]); skexp = cx.tsb("skexp", [128, 32])
    tA = cx.tsb("tA", [128, 4, 64]); tB = cx.tsb("tB", [128, 4, 64])
    qtok = [cx.tsb(f"qtok{i}", [128, 4, 64], BF16) for i in range(2)]
    Pb = [cx.tsb(f"P{i}", [128, 2, 1024], BF16) for i in range(1)] * 2
    den = cx.tsb("den", [128, 512]); rec = cx.tsb("rec", [128, 512])
    gbuf = [cx.tsb(f"g{i}", [128, 2, 512], BF16) for i in range(2)]
    sil = [cx.tsb(f"sil{i}", [128, 512], BF16) for i in range(2)]
    scr = dict(stats=cx.tsb("stats", [128, 4, 6]), mv=cx.tsb("mv", [128, 2]), rstd=cx.tsb("rstd", [128, 1]))
    plebufs = dict(pin=cx.tsb("pin", [128, 4, 256]), pT=cx.tsb("pT", [128, 2, 512], BF16),
                   wp=[cx.tsb(f"wpb{i}", [128, 2, 256], BF16) for i in range(2)], sg=cx.tsb("sg", [128, 256]), tmp=cx.tsb("ptmp", [128, 256]), wa=wa)

    cx.dma(ident.t[:, :], c_ident, wr=[ident]); cx.dma(invf.t[:, :], c_invf, wr=[invf])
    cx.v('dve', 'tensor_copy', identb.t[:, :], ident.t[:, :], rd=[ident], wr=[identb])
    cx.dma(mtmp.t[:, :], c_mprev, wr=[mtmp])
    cx.v('dve', 'tensor_copy', mprev.t[:, :], mtmp.t[:, :], rd=[mtmp], wr=[mprev])
    cx.dma(mtmp.t[:, :], c_mown, wr=[mtmp])
    cx.v('dve', 'tensor_copy', mown.t[:, :], mtmp.t[:, :], rd=[mtmp], wr=[mown])
    cx.dma(posi_o.t[:, :], pos_own, wr=[posi_o]); cx.dma(posi_p.t[:, :], pos_prev, wr=[posi_p])
    cx.dma(pv.t[:, :], pvalid, wr=[pv])
    cx.v('dve', 'memset', ones.t[:, :], 1.0, wr=[ones])
    cx.v('dve', 'tensor_copy', pvmat.t[:, :, :], pv.t[:, :].unsqueeze(2).to_broadcast([128, NT, 128]), rd=[pv], wr=[pvmat])
    cx.dma(sk.t[:, :], sinks.partition_broadcast(128), wr=[sk])
    cx.act(skexp.t[:, :], sk.t[:, :], AF.Exp, rd=[sk], wr=[skexp])
    cx.v('dve', 'memset', Vo.t[:, :, :, :], 0.0, wr=[Vo]); cx.v('dve', 'memset', Vp.t[:, :, :, :], 0.0, wr=[Vp])

    xov = x_own.rearrange("(n p) d -> p n d", p=128)
    xpv = x_prev.rearrange("(n p) d -> p n d", p=128)
    x1v = x1.rearrange("(n p) d -> p n d", p=128)
    p0v = p0.rearrange("(n p) d -> p n d", p=128)
    wqv = wqkv.rearrange("(k p) f -> p k f", p=128)

    for g in range(NG):
        t0 = g * 4
        cx.dma(xo.t[:, :, :], xov[:, t0:t0 + 4, :], wr=[xo])
        rope_tables(cx, posi_o, t0, posf, invf, cos_o, sin_o, sang, sy_, si_, sf_)
        rope_tables(cx, posi_p, t0, posf, invf, cos_p, sin_p, sang, sy_, si_, sf_)
        transpose_group(cx, xo, xo.t, xTa, xTa.t, ident)
        for n in range(4):
            xb_ = xpb[n % 2]
            cx.dma(xb_.t[:, :], xpv[:, t0 + n, :], wr=[xb_], q='pool')
            for k4 in range(4):
                ps = cx.bank()
                psb = ps.t[:, :].bitcast(BF16)
                for kk in range(4):
                    k = k4 * 4 + kk
                    cx.tr(psb[:, kk * 128:(kk + 1) * 128], xb_.t[:, k * 128:(k + 1) * 128], identb.t[:, :],
                          rd=[xb_, identb], wr=[ps], signal=(kk == 3))
                cx.v('dve', 'tensor_copy', xTp.t[:, k4 * 4:(k4 + 1) * 4, n * 128:(n + 1) * 128],
                     psb[:, 0:512].rearrange("p (k t) -> p k t", t=128), rd=[ps], wr=[xTp])
        for s in range(10):
            w = wa[s % 2]
            cx.dma(w.t[:, :, :], wqv[:, :, s * 256:(s + 1) * 256], wr=[w], q='pool')
            for which in ((0, 1) if s >= 8 else (0,)):
                srcT = xTa if which == 0 else xTp
                for n in range(4):
                    ps = cx.bank()
                    for k in range(16):
                        cx.mm(ps.t[:, 0:256], srcT.t[:, k, n * 128:(n + 1) * 128], w.t[:, k, :], k == 0, k == 15,
                              rd=[srcT, w], wr=[ps])
                    if s == 9:
                        Vd = Vo if which == 0 else Vp
                        cx.v('dve', 'tensor_copy', Vd.t[:, n, 0:2, 0:64], ps.t[:, 0:128].rearrange("p (g d) -> p g d", d=64),
                             rd=[ps], wr=[Vd])
                        cx.act(Vd.t[:, n, 2:4, 64:128], ps.t[:, 128:256].rearrange("p (g d) -> p g d", d=64), AF.Copy,
                               rd=[ps], wr=[Vd])
                        continue
                    qt = qtok[(n + s) % 2]
                    cs = (cos_o, sin_o) if which == 0 else (cos_p, sin_p)
                    rope_apply(cx, ps, 4, cs[0], cs[1], n, qt, qt.t[:, :, :], tA, tB)
                    pt = cx.bank()
                    ptb = pt.t[:, :].bitcast(BF16)
                    if s < 8:
                        for hh in range(4):
                            cx.tr(ptb[0:64, hh * 128:(hh + 1) * 128], qt.t[:, hh, :], identb.t[:, :],
                                  rd=[qt, identb], wr=[pt], signal=(hh == 3))
                        h0 = 4 * s
                        half = h0 // 16
                        j0 = h0 % 16
                        dstq = qT.t[half * 64:(half + 1) * 64, j0:j0 + 4, n * 128:(n + 1) * 128]
                        srcq = ptb[0:64, 0:512].rearrange("p (h t) -> p h t", t=128)
                        cx.v('dve', 'tensor_copy', dstq, srcq, rd=[pt], wr=[qT])
                    else:
                        kd = kTo if which == 0 else kTp
                        for hh in range(4):
                            cx.tr(ptb[0:64, hh * 128:(hh + 1) * 128], qt.t[:, hh, :], identb.t[:, :],
                                  rd=[qt, identb], wr=[pt], signal=(hh == 3))
                        for half in range(2):
                            dstk = kd.t[half * 64:(half + 1) * 64, 0:2, n * 128:(n + 1) * 128]
                            srck = ptb[0:64, half * 256:(half + 1) * 256].rearrange("p (h t) -> p h t", t=128)
                            cx.v('dve', 'tensor_copy', dstk, srck, rd=[pt], wr=[kd])
        it = 0
        for n in range(4):
            for gk in range(4):
                half = gk // 2
                pr = slice(half * 64, (half + 1) * 64)
                P = Pb[it % 2]; it += 1
                j0 = (8 * gk) % 16
                for src_i, kd in enumerate((kTp, kTo)):
                    for hq in range(2):
                        ps = cx.bank()
                        rhs = qT.t[pr, j0 + 4 * hq:j0 + 4 * hq + 4, n * 128:(n + 1) * 128]
                        cx.mm(ps.t[:, :], kd.t[pr, gk % 2, n * 128:(n + 1) * 128], rhs, True, True, rd=[kd, qT], wr=[ps])
                        cx.act(P.t[:, src_i, hq * 512:(hq + 1) * 512], ps.t[:, :], AF.Exp, rd=[ps], wr=[P], scale=0.125)
                cx.v('pool', 'tensor_tensor', P.t[:, 0, :].rearrange("p (h t) -> p h t", t=128),
                     P.t[:, 0, :].rearrange("p (h t) -> p h t", t=128),
                     mprev.t[:, :].unsqueeze(1).to_broadcast([128, 8, 128]), ALU.mult, rd=[P, mprev], wr=[P])
                cx.v('pool', 'tensor_tensor', P.t[:, 1, :].rearrange("p (h t) -> p h t", t=128),
                     P.t[:, 1, :].rearrange("p (h t) -> p h t", t=128),
                     mown.t[:, :].unsqueeze(1).to_broadcast([128, 8, 128]), ALU.mult, rd=[P, mown], wr=[P])
                for hq in range(2):
                    po = cx.bank()
                    if half == 0:
                        lp, lo = Vp.t[:, n, gk, 0:64], Vo.t[:, n, gk, 0:64]
                        osl = slice(0, 64)
                    else:
                        lp, lo = Vp.t[:, n, gk, :], Vo.t[:, n, gk, :]
                        osl = slice(0, 128)
                    cx.mm(po.t[osl, :], lp, P.t[:, 0, hq * 512:(hq + 1) * 512], True, False, rd=[Vp, P], wr=[po])
                    cx.mm(po.t[osl, :], lo, P.t[:, 1, hq * 512:(hq + 1) * 512], False, True, rd=[Vo, P], wr=[po])
                    pd = cx.bank()
                    cx.mm(pd.t[:, :], pvmat.t[:, t0 + n, :], P.t[:, 0, hq * 512:(hq + 1) * 512], True, False, rd=[pvmat, P], wr=[pd])
                    cx.mm(pd.t[:, :], ones.t[:, :], P.t[:, 1, hq * 512:(hq + 1) * 512], False, True, rd=[ones, P], wr=[pd])
                    hb = 8 * gk + 4 * hq
                    cx.v('dve', 'tensor_tensor', den.t[pr, :].rearrange("p (h t) -> p h t", t=128),
                         pd.t[pr, :].rearrange("p (h t) -> p h t", t=128),
                         skexp.t[pr, hb:hb + 4].unsqueeze(2).to_broadcast([64, 4, 128]), ALU.add, rd=[pd, skexp], wr=[den])
                    cx.v('dve', 'reciprocal', rec.t[pr, :], den.t[pr, :], rd=[den], wr=[rec])
                    jj = hb % 16
                    cx.v('dve', 'tensor_tensor', attnT.t[pr, jj:jj + 4, n * 128:(n + 1) * 128],
                         po.t[pr, :].rearrange("p (h t) -> p h t", t=128),
                         rec.t[pr, :].rearrange("p (h t) -> p h t", t=128), ALU.mult, rd=[po, rec], wr=[attnT])
        wov = wo.rearrange("(two j d) o -> two d j o", two=2, j=16)
        for s in range(8):
            w = wa[s % 2]
            for two in range(2):
                cx.dma(w.t[two * 64:(two + 1) * 64, :, :], wov[two, :, :, s * 256:(s + 1) * 256], wr=[w], q='pool')
            for n in range(4):
                ps = cx.bank()
                for j in range(16):
                    cx.mm(ps.t[:, 0:256], attnT.t[:, j, n * 128:(n + 1) * 128], w.t[:, j, :], j == 0, j == 15,
                          rd=[attnT, w], wr=[ps])
                dst = xo.t[:, n, s * 256:(s + 1) * 256]
                cx.v('dve', 'scalar_tensor_tensor', dst, dst, ALPHA, ps.t[:, 0:256], ALU.mult, ALU.add, rd=[ps, xo], wr=[xo])
        cx.dma(gB.t[:, :], lng[0:1, :].partition_broadcast(128), wr=[gB])
        cx.dma(bB.t[:, :], lnb[0:1, :].partition_broadcast(128), wr=[bB])
        for n in range(4):
            layer_norm_tile(cx, xo, xo.t[:, n, :], gB, bB, scr)
        transpose_group(cx, xo, xo.t, xTa, xTa.t, ident)
        ffn_slabs(cx, xTa, xTa.t, xo, xo.t, w1, w3, w2, 5632, dict(a=wa, b=wb, c=wc), gbuf, sil, first_scale=ALPHA)
        cx.dma(gB.t[:, :], lng[1:2, :].partition_broadcast(128), wr=[gB])
        cx.dma(bB.t[:, :], lnb[1:2, :].partition_broadcast(128), wr=[bB])
        for n in range(4):
            layer_norm_tile(cx, xo, xo.t[:, n, :], gB, bB, scr)
        transpose_group(cx, xo, xo.t, xTa, xTa.t, ident)
        ple_block(cx, xo, xo.t, xTa, xTa.t, p0v[:, t0:t0 + 4, :], wg, wp, ident, plebufs)
        cx.dma(x1v[:, t0:t0 + 4, :], xo.t[:, :, :], rd=[xo], stream="d_out")
    cx.sy.final_wait('sp', [xo])
    return cx


def rope_apply_n(cx, ps, col0, nh, hd, rd_, cosT, sinT, tile, dst_t, dst, tA, tB):
    half = rd_ // 2
    Xf = ps.t[:, col0:col0 + nh * hd].rearrange("p (h d) -> p h d", d=hd)
    X = Xf[:, :, 0:rd_].rearrange("p h (two d) -> p h two d", two=2)
    cb = cosT.t[:, tile, :].unsqueeze(1).unsqueeze(1).to_broadcast([128, nh, 2, half])
    sbb = sinT.t[:, tile, :].unsqueeze(1).unsqueeze(1).to_broadcast([128, nh, 2, half])
    a = tA.t[:, 0:nh, 0:rd_]
    b = tB.t[:, 0:nh, 0:rd_]
    cx.v('dve', 'tensor_tensor', a.rearrange("p h (two d) -> p h two d", two=2), X, cb, ALU.mult, rd=[ps, cosT], wr=[tA])
    cx.v('dve', 'tensor_tensor', b.rearrange("p h (two d) -> p h two d", two=2), X, sbb, ALU.mult, rd=[ps, sinT], wr=[tB])
    cx.v('pool', 'tensor_tensor', dst[:, :, 0:half], a[:, :, 0:half], b[:, :, half:rd_], ALU.subtract, rd=[tA, tB], wr=[dst_t])
    cx.v('pool', 'tensor_tensor', dst[:, :, half:rd_], a[:, :, half:rd_], b[:, :, 0:half], ALU.add, rd=[tA, tB], wr=[dst_t])
    if rd_ < hd:
        cx.act(dst[:, :, rd_:hd], Xf[:, :, rd_:hd], AF.Copy, rd=[ps], wr=[dst_t])


def build_l2(NT):
    cx = Ctx()
    NG = NT // 4
    TOK = NT * 128
    x1 = cx.din("x1", [TOK, D]); pos_own = cx.din("pos_own", [128, NT], I32)
    w_in = cx.din("w_in", [D, 4752]); kvn = cx.din("kvn", [1, 512]); wuk = cx.din("wuk", [16, 512, 128])
    c_ident = cx.din("c_ident", [128, 128]); c_invf = cx.din("c_invf", [128, 32]); c_invf32 = cx.din("c_invf32", [128, 16])
    kiT_o = cx.dout("kiT_o", [64, TOK]); krT_o = cx.dout("krT_o", [64, TOK])
    ckvT_o = cx.dout("ckvT_o", [128, 4, TOK]); ckv_o = cx.dout("ckv_o", [TOK, 512])
    qiT_o = cx.dout("qiT_o", [NT, 64, 16, 128]); qrT_o = cx.dout("qrT_o", [NT, 64, 16, 128])
    qlatT_o = cx.dout("qlatT_o", [NT, 128, 4, 16, 128]); wi_o = cx.dout("wi_o", [TOK, 16])
    cx.init_psum()
    xo = cx.tsb("xo", [128, 4, D]); xTa = cx.tsb("xTa", [128, 16, 512], BF16)
    wa = [cx.tsb(f"wa{i}", [128, 16, 256], BF16) for i in range(2)]
    ident = cx.tsb("ident", [128, 128]); identb = cx.tsb("identb", [128, 128], BF16)
    invf = cx.tsb("invf", [128, 32]); invf32 = cx.tsb("invf32", [128, 16])
    posi = cx.tsb("posi", [128, NT], I32); posf = cx.tsb("posf", [128, 4])
    cos64 = cx.tsb("cos64", [128, 4, 32]); sin64 = cx.tsb("sin64", [128, 4, 32])
    cos32 = cx.tsb("cos32", [128, 4, 16]); sin32 = cx.tsb("sin32", [128, 4, 16])
    sang = cx.tsb("sang", [128, 4, 32]); sy_ = cx.tsb("sy", [128, 4, 32]); si_ = cx.tsb("si", [128, 4, 32], I32); sf_ = cx.tsb("sf", [128, 4, 32])
    sang2 = cx.tsb("sang2", [128, 4, 16]); sy2 = cx.tsb("sy2", [128, 4, 16]); si2 = cx.tsb("si2", [128, 4, 16], I32); sf2 = cx.tsb("sf2", [128, 4, 16])
    tA = cx.tsb("tA", [128, 4, 64]); tB = cx.tsb("tB", [128, 4, 64])
    qtok = cx.tsb("qtok", [128, 4, 64], BF16)
    stg = [cx.tsb(f"stg{i}", [64, 4, 128]) for i in range(2)]
    wukn = cx.tsb("wukn", [128, 4, 128]); wukT = cx.tsb("wukT", [128, 16, 4, 128], BF16)
    qnT = cx.tsb("qnT", [128, 512], BF16)
    qlst = [cx.tsb(f"qlst{i}", [128, 4, 512]) for i in range(2)]
    kvB = cx.tsb("kvB", [128, 512]); ckr = cx.tsb("ckr", [128, 512]); ckn = cx.tsb("ckn", [128, 512])
    ss = cx.tsb("ss", [128, 1]); junk = cx.tsb("junk", [128, 512])
    ckTs = cx.tsb("ckTs", [128, 4, 128]); wis = cx.tsb("wis", [128, 16])

    cx.dma(ident.t[:, :], c_ident, wr=[ident]); cx.dma(invf.t[:, :], c_invf, wr=[invf]); cx.dma(invf32.t[:, :], c_invf32, wr=[invf32])
    cx.v('dve', 'tensor_copy', identb.t[:, :], ident.t[:, :], rd=[ident], wr=[identb])
    cx.dma(posi.t[:, :], pos_own, wr=[posi])
    cx.dma(kvB.t[:, :], kvn.partition_broadcast(128), wr=[kvB])
    wukv = wuk.rearrange("h (cc p) n -> h p cc n", p=128)
    for h in range(16):
        cx.dma(wukn.t[:, :, :], wukv[h], wr=[wukn])
        ps = cx.bank()
        for cc in range(4):
            cx.tr(ps.t[:, cc * 128:(cc + 1) * 128], wukn.t[:, cc, :], ident.t[:, :], rd=[wukn, ident], wr=[ps], signal=(cc == 3))
        cx.v('dve', 'tensor_copy', wukT.t[:, h, :, :], ps.t[:, :].rearrange("p (cc c) -> p cc c", c=128), rd=[ps], wr=[wukT])

    x1v = x1.rearrange("(n p) d -> p n d", p=128)
    wv = w_in.rearrange("(k p) f -> p k f", p=128)
    ckv_ov = ckv_o.rearrange("(n p) c -> p n c", p=128)
    wi_ov = wi_o.rearrange("(n p) c -> p n c", p=128)
    si = 0
    for g in range(NG):
        t0 = g * 4
        cx.dma(xo.t[:, :, :], x1v[:, t0:t0 + 4, :], wr=[xo])
        rope_tables(cx, posi, t0, posf, invf, cos64, sin64, sang, sy_, si_, sf_)
        rope_tables(cx, posi, t0, posf, invf32, cos32, sin32, sang2, sy2, si2, sf2, nf=16)
        transpose_group(cx, xo, xo.t, xTa, xTa.t, ident)
        for h in range(16):
            w = wa[si % 2]; si += 1
            cx.dma(w.t[:, :, 0:128], wv[:, :, h * 192:h * 192 + 128], wr=[w], q='pool')
            ps = cx.bank()
            for k in range(16):
                cx.mm(ps.t[:, :], w.t[:, k, 0:128], xTa.t[:, k, :], k == 0, k == 15, rd=[w, xTa], wr=[ps])
            cx.act(qnT.t[:, :], ps.t[:, :], AF.Copy, rd=[ps], wr=[qnT])
            ql = qlst[h % 2]
            for cc in range(4):
                p2 = cx.bank()
                cx.mm(p2.t[:, :], wukT.t[:, h, cc, :], qnT.t[:, :], True, True, rd=[wukT, qnT], wr=[p2])
                if cc % 2 == 0:
                    cx.v('dve', 'tensor_copy', ql.t[:, cc, :], p2.t[:, :], rd=[p2], wr=[ql])
                else:
                    cx.act(ql.t[:, cc, :], p2.t[:, :], AF.Copy, rd=[p2], wr=[ql])
            cx.dma(qlatT_o[t0:t0 + 4, :, :, h, :].rearrange("n p cc t -> p cc n t"),
                   ql.t[:, :, :].rearrange("p cc (n t) -> p cc n t", t=128), rd=[ql], stream=f"d_ql{h % 2}")
        wrv = w_in[:, 0:3072].rearrange("(k p) (h f) -> p k h f", p=128, f=192)
        for s in range(4):
            w = wa[si % 2]; si += 1
            for hh in range(4):
                c0 = (4 * s + hh) * 192 + 128
                cx.dma(w.t[:, :, hh * 64:(hh + 1) * 64], wv[:, :, c0:c0 + 64], wr=[w], q='pool')
            for n in range(4):
                ps = cx.bank()
                for k in range(16):
                    cx.mm(ps.t[:, 0:256], xTa.t[:, k, n * 128:(n + 1) * 128], w.t[:, k, :], k == 0, k == 15, rd=[xTa, w], wr=[ps])
                rope_apply_n(cx, ps, 0, 4, 64, 64, cos64, sin64, n, qtok, qtok.t[:, :, :], tA, tB)
                pt = cx.bank(); ptb = pt.t[:, :].bitcast(BF16)
                for hh in range(4):
                    cx.tr(ptb[0:64, hh * 128:(hh + 1) * 128], qtok.t[:, hh, :], identb.t[:, :], rd=[qtok, identb], wr=[pt], signal=(hh == 3))
                st = stg[n % 2]
                cx.v('dve', 'tensor_copy', st.t[:, :, :], ptb[0:64, 0:512].rearrange("p (h t) -> p h t", t=128), rd=[pt], wr=[st])
                cx.dma(qrT_o[t0 + n, :, 4 * s:4 * s + 4, :], st.t[:, :, :], rd=[st], stream=f"d_stg{n % 2}")
        for s in range(4):
            w = wa[si % 2]; si += 1
            cx.dma(w.t[:, :, :], wv[:, :, 3648 + s * 256:3648 + (s + 1) * 256], wr=[w], q='pool')
            for n in range(4):
                ps = cx.bank()
                for k in range(16):
                    cx.mm(ps.t[:, 0:256], xTa.t[:, k, n * 128:(n + 1) * 128], w.t[:, k, :], k == 0, k == 15, rd=[xTa, w], wr=[ps])
                rope_apply_n(cx, ps, 0, 4, 64, 32, cos32, sin32, n, qtok, qtok.t[:, :, :], tA, tB)
                pt = cx.bank(); ptb = pt.t[:, :].bitcast(BF16)
                for hh in range(4):
                    cx.tr(ptb[0:64, hh * 128:(hh + 1) * 128], qtok.t[:, hh, :], identb.t[:, :], rd=[qtok, identb], wr=[pt], signal=(hh == 3))
                st = stg[n % 2]
                cx.v('dve', 'tensor_copy', st.t[:, :, :], ptb[0:64, 0:512].rearrange("p (h t) -> p h t", t=128), rd=[pt], wr=[st])
                cx.dma(qiT_o[t0 + n, :, 4 * s:4 * s + 4, :], st.t[:, :, :], rd=[st], stream=f"d_stg{n % 2}")
        for n in range(4):
            pass
        wck = [None, None]
        for s in range(2):
            w = wa[si % 2]; si += 1
            wck[s] = w
            cx.dma(w.t[:, :, :], wv[:, :, 3072 + s * 256:3072 + (s + 1) * 256], wr=[w], q='pool')
        for n in range(4):
            for s in range(2):
                ps = cx.bank()
                for k in range(16):
                    cx.mm(ps.t[:, 0:256], xTa.t[:, k, n * 128:(n + 1) * 128], wck[s].t[:, k, :], k == 0, k == 15, rd=[xTa, wck[s]], wr=[ps])
                cx.act(ckr.t[:, s * 256:(s + 1) * 256], ps.t[:, 0:256], AF.Copy, rd=[ps], wr=[ckr])
            cx.v('dve', 'tensor_tensor', junk.t[:, :], ckr.t[:, :], ckr.t[:, :], ALU.mult, rd=[ckr], wr=[junk])
            cx.v('dve', 'tensor_reduce', ss.t[:, :], junk.t[:, :], AX.X, ALU.add, rd=[junk], wr=[ss])
            cx.v('dve', 'tensor_scalar', ss.t[:, :], ss.t[:, :], 1.0 / 512, 1e-6, ALU.mult, ALU.add, rd=[ss], wr=[ss])
            cx.act(ss.t[:, :], ss.t[:, :], AF.Sqrt, rd=[ss], wr=[ss])
            cx.v('dve', 'reciprocal', ss.t[:, :], ss.t[:, :], rd=[ss], wr=[ss])
            cx.v('dve', 'scalar_tensor_tensor', ckn.t[:, :], ckr.t[:, :], ss.t[:, 0:1], kvB.t[:, :], ALU.mult, ALU.mult, rd=[ckr, ss, kvB], wr=[ckn])
            cx.dma(ckv_ov[:, t0 + n, :], ckn.t[:, :], rd=[ckn], stream="d_ckn")
            ps = cx.bank()
            for cc in range(4):
                cx.tr(ps.t[:, cc * 128:(cc + 1) * 128], ckn.t[:, cc * 128:(cc + 1) * 128], ident.t[:, :], rd=[ckn, ident], wr=[ps], signal=(cc == 3))
            cx.v('dve', 'tensor_copy', ckTs.t[:, :, :], ps.t[:, :].rearrange("p (cc t) -> p cc t", t=128), rd=[ps], wr=[ckTs])
            cx.dma(ckvT_o[:, :, (t0 + n) * 128:(t0 + n + 1) * 128], ckTs.t[:, :, :], rd=[ckTs], stream="d_ckTs")
        w = wa[si % 2]; si += 1
        cx.dma(w.t[:, :, 0:64], wv[:, :, 3584:3648], wr=[w], q='pool')
        cx.dma(w.t[:, :, 64:144], wv[:, :, 4672:4752], wr=[w], q='pool')
        for n in range(4):
            ps = cx.bank()
            for k in range(16):
                cx.mm(ps.t[:, 0:144], xTa.t[:, k, n * 128:(n + 1) * 128], w.t[:, k, 0:144], k == 0, k == 15, rd=[xTa, w], wr=[ps])
            rope_apply_n(cx, ps, 0, 1, 64, 64, cos64, sin64, n, qtok, qtok.t[:, 0:1, :], tA, tB)
            rope_apply_n(cx, ps, 64, 1, 64, 32, cos32, sin32, n, qtok, qtok.t[:, 1:2, :], tA, tB)
            cx.v('dve', 'tensor_scalar', wis.t[:, :], ps.t[:, 128:144], 0.03125, None, ALU.mult, rd=[ps], wr=[wis])
            cx.dma(wi_ov[:, t0 + n, :], wis.t[:, :], rd=[wis], stream="d_wis")
            pt = cx.bank(); ptb = pt.t[:, :].bitcast(BF16)
            for hh in range(2):
                cx.tr(ptb[0:64, hh * 128:(hh + 1) * 128], qtok.t[:, hh, :], identb.t[:, :], rd=[qtok, identb], wr=[pt], signal=(hh == 1))
            st = stg[n % 2]
            cx.v('dve', 'tensor_copy', st.t[:, 0:2, :], ptb[0:64, 0:256].rearrange("p (h t) -> p h t", t=128), rd=[pt], wr=[st])
            cx.dma(krT_o[:, (t0 + n) * 128:(t0 + n + 1) * 128], st.t[:, 0, :], rd=[st], stream=f"d_stg{n % 2}")
            cx.dma(kiT_o[:, (t0 + n) * 128:(t0 + n + 1) * 128], st.t[:, 1, :], rd=[st], stream=f"d_stg{n % 2}")
    cx.sy.final_wait('sp', qlst + stg + [ckn, ckTs, wis])
    return cx


def barrier(cx):
    sy = cx.sy
    for q, eng in sy.q.items():
        for key, st in list(sy.st.items()):
            c = st[0]
            if c == 0 or sy.seen[q].get(key, 0) >= c:
                continue
            if key == q and q == 'pe':
                continue
            sem, val = sy._sem(key, c)
            eng.wait_ge(sem, val)
            sy.seen[q][key] = c


MLA_SCALE = 192.0 ** -0.5
NIT = 24


def build_l3(NT):
    cx = Ctx()
    nc = cx.nc
    NG = NT // 4
    TOK = NT * 128
    S = 4 * TOK
    SH = S // 2
    x1 = cx.din("x1", [TOK, D]); p1 = cx.din("p1", [TOK, 256])
    kiT_d = cx.din("kiT", [64, S]); krT_d = cx.din("krT", [64, S]); ckvT_d = cx.din("ckvT", [128, 4, S]); ckv_d = cx.din("ckv", [S, 512])
    qiT_d = cx.din("qiT", [NT, 64, 16, 128]); qrT_d = cx.din("qrT", [NT, 64, 16, 128])
    qlatT_d = cx.din("qlatT", [NT, 128, 4, 16, 128]); wi_d = cx.din("wi", [TOK, 16])
    mask4_d = cx.din("mask4", [128, 512]); negb4_d = cx.din("negb4", [128, 512]); c_ident = cx.din("c_ident", [128, 128])
    wuv = cx.din("wuv", [16, 512, 128]); wo1 = cx.din("wo1", [D, D]); wr_d = cx.din("wr", [D, 8])
    mw1 = cx.din("mw1", [8, D, 7168]); mw3 = cx.din("mw3", [8, D, 7168]); mw2 = cx.din("mw2", [8, 7168, D])
    lng = cx.din("lng", [2, D]); lnb = cx.din("lnb", [2, D]); wp = cx.din("wp", [256, D]); wg = cx.din("wg", [D, D])
    out = cx.dout("out", [TOK, D])
    cx.init_psum()
    B = cx.banks
    regA = cx.sb("regA", [128, 32768], BF16)
    score_ap = regA[:, :].bitcast(F32)
    scoreT = [T(score_ap, "score0"), T(score_ap, "score1")]
    def wview(off, shape3):
        return regA[:, off:off + shape3[0] * shape3[1]].rearrange("p (a b) -> p a b", b=shape3[1])
    wa = [T(wview(0, (16, 256)), "wa0"), T(wview(4096, (16, 256)), "wa1")]
    wb = [T(wview(8192, (16, 256)), "wb0"), T(wview(12288, (16, 256)), "wb1")]
    wc = [T(wview(16384, (2, 2048)), "wc0"), T(wview(20480, (2, 2048)), "wc1")]
    xTa = T(wview(24576, (16, 512)), "xTa")
    xo = cx.tsb("xo", [128, 4, D])
    gB = cx.tsb("gB", [128, D]); bB = cx.tsb("bB", [128, D])
    ident = cx.tsb("ident", [128, 128]); identb = cx.tsb("identb", [128, 128], BF16)
    kiT2 = cx.tsb("kiT2", [128, SH], BF16)
    qi2 = cx.tsb("qi2", [128, 16, 128], BF16); wiS = cx.tsb("wiS", [128, 16])
    regB = cx.sb("regB", [128, 10240], BF16)
    qlat = T(regB[:, 0:8192].rearrange("p (a b c) -> p a b c", a=4, b=16), "qlat"); qr = cx.tsb("qr", [64, 16, 128], BF16)
    rbuf = [cx.tsb(f"rbuf{i}", [128, 512]) for i in range(2)]
    mask4 = cx.tsb("mask4s", [128, 512]); negb4 = cx.tsb("negb4s", [128, 512])
    mn = cx.tsb("mn", [128, 1]); mx = cx.tsb("mx", [128, 1]); lo = cx.tsb("lo", [128, 1]); wd = cx.tsb("wd", [128, 1]); mid = cx.tsb("mid", [128, 1])
    cntc = cx.tsb("cntc", [128, 16]); cnt = cx.tsb("cnt", [128, 1]); selv = cx.tsb("selv", [128, 1])
    junk = cx.tsb("junk", [128, 1024], BF16)
    kcT = [cx.tsb(f"kcT{i}", [128, 4, 256], BF16) for i in range(2)]
    kct = [cx.tsb(f"kct{i}", [128, 2, 512], BF16) for i in range(2)]
    kkr = [cx.tsb(f"kkr{i}", [64, 256], BF16) for i in range(2)]
    wuvb = [cx.tsb(f"wuvb{i}", [128, 4, 4, 128], BF16) for i in range(2)]
    selb = [cx.tsb(f"selb{i}", [128, 128], BF16) for i in range(2)]
    Pb = [cx.tsb(f"P{i}", [128, 512], BF16) for i in range(2)]
    ones = cx.tsb("ones", [128, 8], BF16); onesM = cx.tsb("onesM", [128, 128], BF16); rdenb = cx.tsb("rdenb", [128, 512])
    OTs = cx.tsb("OTs", [128, 4, 512], BF16)
    otok = T(regB[:, 8192:10240], "otok"); oT = cx.tsb("oT", [128, 16, 128], BF16)
    wos = [cx.tsb(f"wos{i}", [128, 16, 128], BF16) for i in range(1)] * 2
    gbuf = [T(regB[:, i * 1024:(i + 1) * 1024].rearrange("p (a b) -> p a b", b=512), f"g{i}") for i in range(2)]
    sil = [T(regB[:, 2048 + i * 512:2048 + (i + 1) * 512], f"sil{i}") for i in range(2)]
    scr = dict(stats=cx.tsb("stats", [128, 4, 6]), mv=cx.tsb("mv", [128, 2]), rstd=cx.tsb("rstd", [128, 1]))
    plebufs = dict(pin=T(regB[:, 4096:6144].bitcast(F32).rearrange("p (a b) -> p a b", b=256), "pin"),
                   pT=T(regB[:, 3072:4096].rearrange("p (a b) -> p a b", b=512), "pT"),
                   wp=[cx.tsb(f"wpb{i}", [128, 2, 256], BF16) for i in range(2)], sg=T(regB[:, 6144:6656].bitcast(F32), "sg"),
                   tmp=T(regB[:, 6656:7168].bitcast(F32), "ptmp"), wa=wa)
    wrb = cx.tsb("wrb", [128, 16, 8], BF16); lg = cx.tsb("lg", [128, 4, 8]); m8 = cx.tsb("m8", [128, 8])
    g1 = cx.tsb("gg1", [128, 1]); g2 = cx.tsb("gg2", [128, 1]); ga = cx.tsb("ga", [128, 8]); gb_ = cx.tsb("gb", [128, 8])
    G = cx.tsb("G", [128, 8, 4])

    cx.dma(ident.t[:, :], c_ident, wr=[ident])
    cx.v('dve', 'tensor_copy', identb.t[:, :], ident.t[:, :], rd=[ident], wr=[identb])
    cx.dma(mask4.t[:, :], mask4_d, wr=[mask4]); cx.dma(negb4.t[:, :], negb4_d, wr=[negb4])
    cx.v('dve', 'memset', ones.t[:, :], 1.0, wr=[ones])
    cx.v('dve', 'memset', onesM.t[:, :], 1.0, wr=[onesM])
    cx.dma(kiT2.t[0:64, :], kiT_d[:, 0:SH], wr=[kiT2], q='pool')
    cx.dma(kiT2.t[64:128, :], kiT_d[:, SH:S], wr=[kiT2], q='pool')
    cx.dma(wrb.t[:, :, :], wr_d.rearrange("(k p) e -> p k e", p=128), wr=[wrb], q='pool')

    x1v = x1.rearrange("(n p) d -> p n d", p=128)
    outv = out.rearrange("(n p) d -> p n d", p=128)
    p1v = p1.rearrange("(n p) d -> p n d", p=128)
    wiv = wi_d.rearrange("(n p) c -> p n c", p=128)
    ckvv = ckv_d.rearrange("(n p) c -> p n c", p=128)
    wuvv = wuv.rearrange("h (cc p) v -> p h cc v", p=128)
    wo1v = wo1.rearrange("(k p) o -> p k o", p=128)
    OT = B[0:4]; DEN = B[4]; PL = [B[5], B[6]]; PM = B[7]
    pmb = PM.t[:, :].bitcast(BF16)

    for g in range(NG):
        for n in range(4):
            i = g * 4 + n
            nS = 4 * i + 4
            NK = nS * 128
            cx.dma(xo.t[:, n, :], x1v[:, i, :], wr=[xo])
            cx.dma(qi2.t[0:64, :, :], qiT_d[i], wr=[qi2], q='pool')
            cx.dma(qi2.t[64:128, :, :], qiT_d[i], wr=[qi2], q='pool')
            cx.dma(wiS.t[:, :], wiv[:, i, :], wr=[wiS])
            cx.dma(regB[:, 0:8192], qlatT_d[i].rearrange("p a b c -> p (a b c)"), wr=[qlat], q='pool')
            cx.dma(qr.t[:, :, :], qrT_d[i], wr=[qr], q='pool')
            for c4 in range(nS // 4):
                half = 1 if c4 * 512 >= SH else 0
                col = c4 * 512 - half * SH
                pr = slice(half * 64, (half + 1) * 64)
                sT = scoreT[c4 % 2]
                eng = 'dve'
                sc = score_ap[:, c4 * 512:(c4 + 1) * 512]
                for h in range(16):
                    ps = cx.bank()
                    cx.mm(ps.t[:, :], qi2.t[pr, h, :], kiT2.t[pr, col:col + 512], True, True, rd=[qi2, kiT2], wr=[ps])
                    r = rbuf[h % 2]
                    cx.act(r.t[:, :], ps.t[:, :], AF.Relu, rd=[ps], wr=[r])
                    if h == 0:
                        cx.v(eng, 'tensor_scalar', sc, r.t[:, :], wiS.t[:, 0:1], None, ALU.mult, rd=[r, wiS], wr=[sT])
                    else:
                        cx.v(eng, 'scalar_tensor_tensor', sc, r.t[:, :], wiS.t[:, h:h + 1], sc, ALU.mult, ALU.add, rd=[r, wiS, sT], wr=[sT])
            sall = score_ap[:, 0:NK]
            cx.v('dve', 'tensor_reduce', mn.t[:, :], sall, AX.X, ALU.min, rd=scoreT, wr=[mn])
            last = score_ap[:, NK - 512:NK]
            cx.v('dve', 'tensor_tensor', last, last, mask4.t[:, :], ALU.mult, rd=scoreT + [mask4], wr=scoreT)
            cx.v('dve', 'tensor_tensor', last, last, negb4.t[:, :], ALU.add, rd=scoreT + [negb4], wr=scoreT)
            cx.v('dve', 'tensor_reduce', mx.t[:, :], sall, AX.X, ALU.max, rd=scoreT, wr=[mx])
            cx.v('dve', 'tensor_scalar', lo.t[:, :], mn.t[:, :], -1.0, None, ALU.add, rd=[mn], wr=[lo])
            cx.v('dve', 'tensor_tensor', wd.t[:, :], mx.t[:, :], lo.t[:, :], ALU.subtract, rd=[mx, lo], wr=[wd])
            CH = 1024
            nch = (NK + CH - 1) // CH
            for it in range(NIT):
                cx.v('dve', 'tensor_scalar', wd.t[:, :], wd.t[:, :], 0.5, None, ALU.mult, rd=[wd], wr=[wd])
                cx.v('dve', 'tensor_tensor', mid.t[:, :], lo.t[:, :], wd.t[:, :], ALU.add, rd=[lo, wd], wr=[mid])
                for ch in range(nch):
                    c0 = ch * CH; c1 = min(NK, c0 + CH)
                    cx.v('dve', 'tensor_scalar', junk.t[:, 0:c1 - c0], score_ap[:, c0:c1], mid.t[:, 0:1], None, ALU.is_gt, ALU.add,
                         rd=scoreT + [mid], wr=[junk, cntc], accum_out=cntc.t[:, ch:ch + 1])
                cx.v('dve', 'tensor_reduce', cnt.t[:, :], cntc.t[:, 0:nch], AX.X, ALU.add, rd=[cntc], wr=[cnt])
                cx.v('dve', 'tensor_scalar', selv.t[:, :], cnt.t[:, :], 255.5, None, ALU.is_gt, rd=[cnt], wr=[selv])
                cx.v('dve', 'scalar_tensor_tensor', lo.t[:, :], wd.t[:, :], selv.t[:, 0:1], lo.t[:, :], ALU.mult, ALU.add,
                     rd=[wd, selv, lo], wr=[lo])
            kbi = 0
            for hg in range(4):
                wv_ = wuvb[hg % 2]
                cx.dma(wv_.t[:, :, :, :], wuvv[:, hg * 4:(hg + 1) * 4, :, :], wr=[wv_], q='pool')
                for S2 in range(nS // 2):
                    kb = kbi % 2; kbi += 1
                    k0 = S2 * 256
                    cx.dma(kcT[kb].t[:, :, :], ckvT_d[:, :, k0:k0 + 256], wr=[kcT[kb]], q='pool')
                    cx.dma(kct[kb].t[:, :, :], ckvv[:, S2 * 2:S2 * 2 + 2, :], wr=[kct[kb]], q='pool')
                    cx.dma(kkr[kb].t[:, :], krT_d[:, k0:k0 + 256], wr=[kkr[kb]], q='pool')
                    for ss in range(2):
                        St = S2 * 2 + ss
                        sb_ = selb[St % 2]
                        cx.v('dve', 'tensor_scalar', sb_.t[:, :], score_ap[:, St * 128:(St + 1) * 128], lo.t[:, 0:1], None, ALU.is_gt,
                             rd=scoreT + [lo], wr=[sb_])
                        mslot = St % 8
                        pm = pmb[:, mslot * 128:(mslot + 1) * 128]
                        cx.tr(pm, sb_.t[:, :], identb.t[:, :], rd=[sb_, identb], wr=[PM])
                        pl = PL[St % 2]
                        for cc in range(4):
                            cx.mm(pl.t[:, :], kcT[kb].t[:, cc, ss * 128:(ss + 1) * 128], qlat.t[:, cc, hg * 4:(hg + 1) * 4, :],
                                  cc == 0, False, rd=[kcT[kb], qlat], wr=[pl])
                        cx.mm(pl.t[:, :], kkr[kb].t[:, ss * 128:(ss + 1) * 128], qr.t[:, hg * 4:(hg + 1) * 4, :], False, True,
                              rd=[kkr[kb], qr], wr=[pl])
                        P = Pb[St % 2]
                        cx.act(P.t[:, :], pl.t[:, :], AF.Exp, rd=[pl], wr=[P], scale=MLA_SCALE)
                        P3 = P.t[:, :].rearrange("p (h t) -> p h t", t=128)
                        cx.v('dve', 'tensor_tensor', P3, P3, pm.unsqueeze(1).to_broadcast([128, 4, 128]), ALU.mult, rd=[P, PM], wr=[P])
                        for cc in range(4):
                            cx.mm(OT[cc].t[:, :], kct[kb].t[:, ss, cc * 128:(cc + 1) * 128], P.t[:, :], St == 0, St == nS - 1,
                                  rd=[kct[kb], P], wr=[OT[cc]])
                        cx.mm(DEN.t[:, :], onesM.t[:, :], P.t[:, :], St == 0, St == nS - 1, rd=[P, onesM], wr=[DEN])
                cx.v('dve', 'reciprocal', rdenb.t[:, :], DEN.t[:, :], rd=[DEN], wr=[rdenb])
                for cc in range(4):
                    cx.v('dve', 'tensor_tensor', OTs.t[:, cc, :], OT[cc].t[:, :], rdenb.t[:, :], ALU.mult, rd=[OT[cc], rdenb], wr=[OTs])
                po = PL[0]
                for h in range(4):
                    for cc in range(4):
                        cx.mm(po.t[:, h * 128:(h + 1) * 128], OTs.t[:, cc, h * 128:(h + 1) * 128], wv_.t[:, h, cc, :], cc == 0, cc == 3,
                              rd=[OTs, wv_], wr=[po])
                for h in range(4):
                    hgl = hg * 4 + h
                    cx.act(otok.t[:, hgl * 128:(hgl + 1) * 128], po.t[:, h * 128:(h + 1) * 128], AF.Copy, rd=[po], wr=[otok])
            for k4 in range(2):
                ps = cx.bank(); psb = ps.t[:, :].bitcast(BF16)
                for kk in range(8):
                    k = k4 * 8 + kk
                    cx.tr(psb[:, kk * 128:(kk + 1) * 128], otok.t[:, k * 128:(k + 1) * 128], identb.t[:, :], rd=[otok, identb], wr=[ps],
                          signal=(kk == 7))
                cx.v('dve', 'tensor_copy', oT.t[:, k4 * 8:(k4 + 1) * 8, :], psb[:, 0:1024].rearrange("p (k t) -> p k t", t=128), rd=[ps], wr=[oT])
            for s in range(16):
                w = wos[s % 2]
                cx.dma(w.t[:, :, :], wo1v[:, :, s * 128:(s + 1) * 128], wr=[w], q='pool')
                ps = cx.bank()
                for k in range(16):
                    cx.mm(ps.t[:, 0:128], oT.t[:, k, :], w.t[:, k, :], k == 0, k == 15, rd=[oT, w], wr=[ps])
                dst = xo.t[:, n, s * 128:(s + 1) * 128]
                cx.v('dve', 'scalar_tensor_tensor', dst, dst, ALPHA, ps.t[:, 0:128], ALU.mult, ALU.add, rd=[ps, xo], wr=[xo])
            if n == 0:
                cx.dma(gB.t[:, :], lng[0:1, :].partition_broadcast(128), wr=[gB])
                cx.dma(bB.t[:, :], lnb[0:1, :].partition_broadcast(128), wr=[bB])
            layer_norm_tile(cx, xo, xo.t[:, n, :], gB, bB, scr)
        barrier(cx)
        transpose_group(cx, xo, xo.t, xTa, xTa.t, ident)
        for n in range(4):
            ps = cx.bank()
            for k in range(16):
                cx.mm(ps.t[:, 0:8], xTa.t[:, k, n * 128:(n + 1) * 128], wrb.t[:, k, :], k == 0, k == 15, rd=[xTa, wrb], wr=[ps])
            cx.v('dve', 'tensor_copy', lg.t[:, n, :], ps.t[:, 0:8], rd=[ps], wr=[lg])
            cx.v('dve', 'max', m8.t[:, :], lg.t[:, n, :], rd=[lg], wr=[m8])
            cx.v('dve', 'tensor_tensor', g1.t[:, :], m8.t[:, 1:2], m8.t[:, 0:1], ALU.subtract, rd=[m8], wr=[g1])
            cx.act(g1.t[:, :], g1.t[:, :], AF.Exp, rd=[g1], wr=[g1])
            cx.v('dve', 'tensor_scalar', g1.t[:, :], g1.t[:, :], 1.0, None, ALU.add, rd=[g1], wr=[g1])
            cx.v('dve', 'reciprocal', g1.t[:, :], g1.t[:, :], rd=[g1], wr=[g1])
            cx.v('dve', 'tensor_scalar', g2.t[:, :], g1.t[:, :], -1.0, 1.0, ALU.mult, ALU.add, rd=[g1], wr=[g2])
            cx.v('dve', 'tensor_scalar', ga.t[:, :], lg.t[:, n, :], m8.t[:, 0:1], g1.t[:, 0:1], ALU.is_equal, ALU.mult, rd=[lg, m8, g1], wr=[ga])
            cx.v('dve', 'tensor_scalar', gb_.t[:, :], lg.t[:, n, :], m8.t[:, 1:2], g2.t[:, 0:1], ALU.is_equal, ALU.mult, rd=[lg, m8, g2], wr=[gb_])
            cx.v('dve', 'tensor_tensor', G.t[:, :, n], ga.t[:, :], gb_.t[:, :], ALU.add, rd=[ga, gb_], wr=[G])
        for e in range(8):
            ffn_slabs(cx, xTa, xTa.t, xo, xo.t, mw1[e], mw3[e], mw2[e], 7168, dict(a=wa, b=wb, c=wc), gbuf, sil,
                      gates=G.t[:, e, :], gates_t=G, first_scale=(ALPHA if e == 0 else None))
        cx.dma(gB.t[:, :], lng[1:2, :].partition_broadcast(128), wr=[gB])
        cx.dma(bB.t[:, :], lnb[1:2, :].partition_broadcast(128), wr=[bB])
        for n in range(4):
            layer_norm_tile(cx, xo, xo.t[:, n, :], gB, bB, scr)
        transpose_group(cx, xo, xo.t, xTa, xTa.t, ident)
        ple_block(cx, xo, xo.t, xTa, xTa.t, p1v[:, g * 4:g * 4 + 4, :], wg, wp, ident, plebufs)
        cx.dma(outv[:, g * 4:g * 4 + 4, :], xo.t[:, :, :], rd=[xo], stream="d_out")
        barrier(cx)
    cx.sy.final_wait('sp', [xo])
    return cx


def consts():
    ident = np.eye(128, dtype=np.float32)
    invf = np.tile((10000.0 ** (-np.arange(0, 64, 2, dtype=np.float32) / 64)).astype(np.float32)[None, :], (128, 1))
    invf32 = np.tile((10000.0 ** (-np.arange(0, 32, 2, dtype=np.float32) / 32)).astype(np.float32)[None, :], (128, 1))
    s = np.arange(128)[:, None]; t = np.arange(128)[None, :]
    mprev = (s > t).astype(np.float32); mown = (s <= t).astype(np.float32)
    return dict(c_ident=ident, c_invf=invf, c_invf32=invf32, c_mprev=mprev, c_mown=mown)
def own_tiles(a, j, NT):
    t = a.reshape((4 * NT, 128) + a.shape[1:])[j::4]
    return np.ascontiguousarray(t.reshape((NT * 128,) + a.shape[1:]))
def pos_layout(pos_b, idx):
    pt = pos_b.reshape(-1, 128)
    return np.ascontiguousarray(pt[idx].T).astype(np.int32)
def l1_maps(inp, NT):
    C = consts(); maps = []
    for c in range(8):
        b, j = c // 4, c % 4
        xt = inp['x'][b].reshape(4 * NT, 128, 2048)
        prev_idx = np.arange(j, 4 * NT, 4) - 1
        prev = xt[np.maximum(prev_idx, 0)].copy()
        pvalid = np.ones((128, NT), np.float32)
        if prev_idx[0] < 0:
            prev[0] = 0; pvalid[:, 0] = 0
        maps.append(dict(x_own=own_tiles(inp['x'][b], j, NT), x_prev=np.ascontiguousarray(prev.reshape(NT * 128, 2048)),
                 pos_own=pos_layout(inp['positions'][b], np.arange(j, 4 * NT, 4)), pos_prev=pos_layout(inp['positions'][b], np.maximum(prev_idx, 0)),
                 pvalid=pvalid, p0=own_tiles(inp['p'][0, b], j, NT),
                 wqkv=inp['swa_w_qkv'][0], sinks=inp['swa_sinks'][0:1], wo=inp['swa_w_o'][0],
                 w1=inp['dense_w1'][0], w3=inp['dense_w3'][0], w2=inp['dense_w2'][0],
                 lng=inp['ln_g'][0], lnb=inp['ln_b'][0], wp=inp['ple_w_p'][0], wg=inp['ple_w_g'][0],
                 c_ident=C['c_ident'], c_invf=C['c_invf'], c_mprev=C['c_mprev'], c_mown=C['c_mown']))
    return maps
def l2_maps(inp, x1_cores, NT):
    C = consts(); maps = []
    for c in range(8):
        b, j = c // 4, c % 4
        maps.append(dict(x1=x1_cores[c], pos_own=pos_layout(inp['positions'][b], np.arange(j, 4 * NT, 4)),
                         w_in=inp['dsa_w_in'][0], kvn=inp['dsa_kv_norm'][0:1], wuk=inp['dsa_w_uk'][0],
                         c_ident=C['c_ident'], c_invf=C['c_invf'], c_invf32=C['c_invf32']))
    return maps
def l3_maps(inp, x1_cores, r2, NT):
    C = consts(); maps = []
    S = 4 * NT * 128
    full = []
    for b in range(2):
        kiT = np.zeros((64, S), np.float32); krT = np.zeros((64, S), np.float32)
        ckvT = np.zeros((128, 4, S), np.float32); ckv = np.zeros((S, 512), np.float32)
        for j in range(4):
            r = r2[4 * b + j]
            kiT.reshape(64, 4 * NT, 128)[:, j::4] = r['kiT_o'].reshape(64, NT, 128)
            krT.reshape(64, 4 * NT, 128)[:, j::4] = r['krT_o'].reshape(64, NT, 128)
            ckvT.reshape(128, 4, 4 * NT, 128)[:, :, j::4] = r['ckvT_o'].reshape(128, 4, NT, 128)
            ckv.reshape(4 * NT, 128, 512)[j::4] = r['ckv_o'].reshape(NT, 128, 512)
        full.append((kiT, krT, ckvT, ckv))
    for c in range(8):
        b, j = c // 4, c % 4
        r = r2[c]
        m4 = np.zeros((128, 512), np.float32)
        t = np.arange(128)[:, None]; s = np.arange(128)[None, :]
        for kk in range(4):
            if kk < j: m4[:, kk * 128:(kk + 1) * 128] = 1
            elif kk == j: m4[:, kk * 128:(kk + 1) * 128] = (s <= t)
        nb4 = ((m4 - 1) * 1e30).astype(np.float32)
        kiT, krT, ckvT, ckv = full[b]
        maps.append(dict(x1=x1_cores[c], p1=own_tiles(inp['p'][1, b], j, NT), kiT=kiT, krT=krT, ckvT=ckvT, ckv=ckv,
                         qiT=r['qiT_o'], qrT=r['qrT_o'], qlatT=r['qlatT_o'], wi=r['wi_o'], mask4=m4, negb4=nb4, c_ident=C['c_ident'],
                         wuv=inp['dsa_w_uv'][0], wo1=inp['dsa_w_o'][0], wr=inp['moe_router'][0],
                         mw1=inp['moe_w1'][0], mw3=inp['moe_w3'][0], mw2=inp['moe_w2'][0],
                         lng=inp['ln_g'][1], lnb=inp['ln_b'][1], wp=inp['ple_w_p'][1], wg=inp['ple_w_g'][1]))
    return maps
def assemble(res_list, key, NT):
    out = np.zeros((2, 4 * NT * 128, 2048), np.float32)
    for c in range(8):
        b, j = c // 4, c % 4
        out[b].reshape(4 * NT, 128, 2048)[j::4] = np.asarray(res_list[c][key]).reshape(NT, 128, 2048)
    return out


from concourse.bass_utils import run_bass_kernel_spmd

NT_FULL = 32


def kernel(**inputs):
    inp = {k: np.asarray(v) for k, v in inputs.items()}
    NT = NT_FULL
    cores = list(range(8))
    cx1 = build_l1(NT)
    res1 = run_bass_kernel_spmd(cx1.nc, l1_maps(inp, NT), core_ids=cores)
    x1_cores = [np.asarray(r['x1']) for r in res1.results]
    del res1, cx1
    cx2 = build_l2(NT)
    res2 = run_bass_kernel_spmd(cx2.nc, l2_maps(inp, x1_cores, NT), core_ids=cores)
    r2 = [{k: np.asarray(v) for k, v in r.items()} for r in res2.results]
    del res2, cx2
    cx3 = build_l3(NT)
    res3 = run_bass_kernel_spmd(cx3.nc, l3_maps(inp, x1_cores, r2, NT), core_ids=cores)
    out = assemble(res3.results, 'out', NT)
    return out.astype(np.float32)
```
